# Optimizing a Trainium2 kernel written in Bass

```python
import numpy as np
import jax, jax.numpy as jnp
from jax import lax

D_MODEL = 1024
BATCH = 4
SEQ = 8192
DEPTH = 1

ATT_HEADS = 8
ATT_HEAD_DIM = 64
ATT_KV_GROUPS = 2
ATT_HPG = ATT_HEADS // ATT_KV_GROUPS
ATT_WIDTH = ATT_HEADS * ATT_HEAD_DIM
KV_WIDTH = ATT_KV_GROUPS * ATT_HEAD_DIM
N_BRANCH = 3
L_CMP = 32
STRIDE_CMP = 16
CMP_HIDDEN = 256
L_SEL = 64
N_SELECT = 16
N_FORCED_LOCAL = 2
FORCE_BONUS = 1000.0
WINDOW = 512
Q_BLOCK = 128
ROPE_THETA = 500000.0
ROPE_DIM = ATT_HEAD_DIM // 4

HG_HEADS = 4
HG_DK = 128
HG_DV = 128
HG_WIDTH = HG_HEADS * HG_DV
HG_CHUNK = 64

MIX_WIDTH = ATT_WIDTH + HG_WIDTH
IN_SIZES = (ATT_WIDTH, KV_WIDTH, KV_WIDTH, KV_WIDTH, KV_WIDTH, KV_WIDTH, KV_WIDTH,
            ATT_HEADS * N_BRANCH, HG_HEADS * HG_DK, HG_HEADS * HG_DK, HG_WIDTH, HG_WIDTH)
IN_WIDTH = sum(IN_SIZES)

N_EXPERTS = 32
TOP_K = 4
D_FF = 1024
SWIGLU_LIMIT = 7.0
SWIGLU_ALPHA = 1.702
MOE_BLOCK = 256

DN_ALPHA = (2 * DEPTH) ** 0.25
DN_BETA = (8 * DEPTH) ** -0.25
LN_EPS = 1e-5
RMS_EPS = 1e-6

kernel_name = "hymba_nsa_hgrn2_moe_deepnorm"

F32 = jnp.float32


def layer_norm(x, g, b):
    xf = x.astype(F32)
    mu = jnp.mean(xf, axis=-1, keepdims=True)
    var = jnp.mean(jnp.square(xf - mu), axis=-1, keepdims=True)
    return ((xf - mu) * lax.rsqrt(var + LN_EPS) * g.astype(F32) + b.astype(F32)).astype(x.dtype)


def rope_partial(x, pos):
    half = ROPE_DIM // 2
    inv = ROPE_THETA ** (-jnp.arange(0, ROPE_DIM, 2, dtype=F32) / ROPE_DIM)
    ang = pos.astype(F32)[..., None] * inv
    cos = jnp.cos(ang)[:, :, None, :]
    sin = jnp.sin(ang)[:, :, None, :]
    xr = x[..., :ROPE_DIM].astype(F32)
    x1, x2 = xr[..., :half], xr[..., half:]
    rot = jnp.concatenate([x1 * cos - x2 * sin, x2 * cos + x1 * sin], axis=-1).astype(x.dtype)
    return jnp.concatenate([rot, x[..., ROPE_DIM:]], axis=-1)


def masked_softmax(s, mask):
    s = jnp.where(mask, s, -1e30)
    m = jnp.max(s, axis=-1, keepdims=True)
    p = jnp.exp(s - m) * mask
    return p / jnp.maximum(jnp.sum(p, axis=-1, keepdims=True), 1e-20)


def nsa_attention(q, k_cmp, v_cmp, k_sel, v_sel, k_win, v_win, gate_logits, pos,
                  pe_cmp, w_ck1, w_ck2, w_cv1, w_cv2):
    B, S = q.shape[0], q.shape[1]
    G, dh = ATT_KV_GROUPS, ATT_HEAD_DIM
    scale = dh ** -0.5
    q = rope_partial(q.reshape(B, S, ATT_HEADS, dh), pos).reshape(B, S, G, ATT_HPG, dh)
    k_cmp, v_cmp, k_sel, v_sel, k_win, v_win = [t.reshape(B, S, G, dh) for t in
                                                (k_cmp, v_cmp, k_sel, v_sel, k_win, v_win)]
    k_sel = rope_partial(k_sel, pos)
    k_win = rope_partial(k_win, pos)

    n_cmp = (S - L_CMP) // STRIDE_CMP + 1
    blk_idx = np.arange(n_cmp)[:, None] * STRIDE_CMP + np.arange(L_CMP)[None, :]
    cmp_end = blk_idx[:, -1]

    def compress(t, w1, w2):
        tb = t[:, blk_idx] + pe_cmp[None, None, :, None, :]
        tb = jnp.moveaxis(tb, 3, 2).reshape(B, n_cmp, G, L_CMP * dh)
        return jax.nn.gelu(tb @ w1) @ w2

    kc = rope_partial(compress(k_cmp, w_ck1, w_ck2), pos[:, cmp_end])
    vc = compress(v_cmp, w_cv1, w_cv2)

    n_blk = S // L_SEL
    n_pick = min(N_SELECT, n_blk)
    kbl = k_sel.reshape(B, n_blk, L_SEL, G, dh).transpose(0, 3, 1, 2, 4)
    vbl = v_sel.reshape(B, n_blk, L_SEL, G, dh).transpose(0, 3, 1, 2, 4)
    cs = np.arange(n_cmp) * STRIDE_CMP
    ss = np.arange(n_blk) * L_SEL
    overlap = jnp.asarray(((cs[:, None] < ss[None, :] + L_SEL) &
                           (ss[None, :] < cs[:, None] + L_CMP)).astype(np.float32))
    gather = jax.vmap(jax.vmap(lambda tb, ix: tb[ix]))

    kwp = jnp.pad(k_win, ((0, 0), (WINDOW, 0), (0, 0), (0, 0)))
    vwp = jnp.pad(v_win, ((0, 0), (WINDOW, 0), (0, 0), (0, 0)))
    gates = jax.nn.sigmoid(gate_logits.astype(F32)).reshape(B, S, G, ATT_HPG, N_BRANCH)
    blk_ids = jnp.arange(n_blk)

    def block(qb):
        s0 = qb * Q_BLOCK
        qblk = lax.dynamic_slice_in_dim(q, s0, Q_BLOCK, axis=1)
        t_pos = s0 + jnp.arange(Q_BLOCK)

        s = jnp.einsum('btgnd,bcgd->bgntc', qblk, kc, preferred_element_type=F32) * scale
        p_c = masked_softmax(s, cmp_end[None, :] <= t_pos[:, None])
        o_c = jnp.einsum('bgntc,bcgd->btgnd', p_c.astype(vc.dtype), vc)

        imp = jnp.einsum('bgntc,cj->bgtj', p_c, overlap)
        cur = t_pos // L_SEL
        dist = cur[:, None] - blk_ids[None, :]
        causal_blk = dist >= 0
        forced = (blk_ids[None, :] == 0) | (causal_blk & (dist < N_FORCED_LOCAL))
        imp = jnp.where(causal_blk, imp + FORCE_BONUS * forced, -1.0)
        _, sel = lax.top_k(imp, n_pick)
        ks = gather(kbl, sel).reshape(B, G, Q_BLOCK, n_pick * L_SEL, dh)
        vs = gather(vbl, sel).reshape(B, G, Q_BLOCK, n_pick * L_SEL, dh)
        kpos = (sel[..., None] * L_SEL + jnp.arange(L_SEL)).reshape(B, G, Q_BLOCK, n_pick * L_SEL)
        s = jnp.einsum('btgnd,bgtkd->bgntk', qblk, ks, preferred_element_type=F32) * scale
        p_s = masked_softmax(s, (kpos <= t_pos[None, None, :, None])[:, :, None])
        o_s = jnp.einsum('bgntk,bgtkd->btgnd', p_s.astype(vs.dtype), vs)

        kw = lax.dynamic_slice_in_dim(kwp, s0, WINDOW + Q_BLOCK, axis=1)
        vw = lax.dynamic_slice_in_dim(vwp, s0, WINDOW + Q_BLOCK, axis=1)
        kp = s0 - WINDOW + jnp.arange(WINDOW + Q_BLOCK)
        m_w = ((kp[None, :] <= t_pos[:, None]) & (kp[None, :] > t_pos[:, None] - WINDOW)
               & (kp[None, :] >= 0))
        s = jnp.einsum('btgnd,bkgd->bgntk', qblk, kw, preferred_element_type=F32) * scale
        p_w = masked_softmax(s, m_w)
        o_w = jnp.einsum('bgntk,bkgd->btgnd', p_w.astype(vw.dtype), vw)

        gb = lax.dynamic_slice_in_dim(gates, s0, Q_BLOCK, axis=1)
        o = gb[..., 0:1] * o_c + gb[..., 1:2] * o_s + gb[..., 2:3] * o_w
        return o.astype(q.dtype)

    out = lax.map(block, jnp.arange(S // Q_BLOCK))
    return jnp.moveaxis(out, 0, 1).reshape(B, S, ATT_WIDTH)


def hgrn2(q, zf, inp, g, lb, norm_g):
    B, S = q.shape[0], q.shape[1]
    H, dk, dv, C = HG_HEADS, HG_DK, HG_DV, HG_CHUNK
    f_hat = jax.nn.sigmoid(zf.astype(F32))
    logf = jnp.log(lb + (1.0 - lb) * f_hat)
    k = (1.0 - lb) * (1.0 - f_hat)

    def heads(t, d):
        return t.reshape(B, S // C, C, H, d).transpose(1, 0, 3, 2, 4)

    qs, ks, ls, vs = heads(q.astype(F32), dk), heads(k, dk), heads(logf, dk), heads(inp.astype(F32), dv)
    causal = jnp.asarray(np.tril(np.ones((C, C), dtype=bool)))[:, :, None]

    def step(state, xs):
        qc, kc, lc, vc = xs
        b = jnp.cumsum(lc, axis=2)
        o_inter = jnp.einsum('bhtk,bhkv->bhtv', qc * jnp.exp(b), state)
        diff = b[:, :, :, None, :] - b[:, :, None, :, :]
        decay = jnp.where(causal, jnp.exp(jnp.where(causal, diff, 0.0)), 0.0)
        A = jnp.einsum('bhtk,bhsk,bhtsk->bhts', qc, kc, decay)
        o = o_inter + jnp.einsum('bhts,bhsv->bhtv', A, vc)
        b_last = b[:, :, -1]
        state = (jnp.exp(b_last)[..., None] * state +
                 jnp.einsum('bhsk,bhsv->bhkv', kc * jnp.exp(b_last[:, :, None] - b), vc))
        return state, o

    state0 = jnp.zeros((B, H, dk, dv), F32)
    _, o = lax.scan(step, state0, (qs, ks, ls, vs))
    o = o.transpose(1, 0, 3, 2, 4).reshape(B, S, H, dv)
    o = o * lax.rsqrt(jnp.mean(jnp.square(o), axis=-1, keepdims=True) + RMS_EPS) * norm_g.astype(F32)
    o = o.reshape(B, S, HG_WIDTH) * jax.nn.silu(g.astype(F32))
    return o.astype(q.dtype)


def moe(x, w_r, b_r, w_gu, b_gu, w_dn, b_dn):
    B, S, D = x.shape
    N = B * S
    A = N * TOP_K
    xf = x.reshape(N, D)
    logits = (xf @ w_r + b_r).astype(F32)
    top_v, top_i = lax.top_k(logits, TOP_K)
    top_w = jax.nn.softmax(top_v, axis=-1)
    e_flat = top_i.reshape(A)
    tok_flat = jnp.arange(A) // TOP_K
    w_flat = top_w.reshape(A)
    order = jnp.argsort(e_flat)
    e_sorted, tok_sorted, w_sorted = e_flat[order], tok_flat[order], w_flat[order]
    counts = jnp.bincount(e_flat, length=N_EXPERTS)
    padded = (counts + MOE_BLOCK - 1) // MOE_BLOCK * MOE_BLOCK
    starts = jnp.cumsum(counts) - counts
    pends = jnp.cumsum(padded)
    pstarts = pends - padded
    dest = pstarts[e_sorted] + jnp.arange(A) - starts[e_sorted]
    n_blk = -(-(A + N_EXPERTS * (MOE_BLOCK - 1)) // MOE_BLOCK)
    P = n_blk * MOE_BLOCK
    row_tok = jnp.zeros((P,), jnp.int32).at[dest].set(tok_sorted)
    row_w = jnp.zeros((P,), F32).at[dest].set(w_sorted)
    blk_expert = jnp.clip(jnp.searchsorted(pends, jnp.arange(n_blk) * MOE_BLOCK, side='right'),
                          0, N_EXPERTS - 1)
    xs = xf[row_tok].reshape(n_blk, MOE_BLOCK, D)

    def expert_block(args):
        xb, e = args
        h = xb @ w_gu[e] + b_gu[e]
        gate = jnp.minimum(h[:, :D_FF], SWIGLU_LIMIT)
        up = jnp.clip(h[:, D_FF:], -SWIGLU_LIMIT, SWIGLU_LIMIT)
        act = gate * jax.nn.sigmoid(SWIGLU_ALPHA * gate) * (up + 1.0)
        return act @ w_dn[e] + b_dn[e]

    ys = lax.map(expert_block, (xs, blk_expert)).reshape(P, D)
    out = jnp.zeros((N, D), F32).at[row_tok].add(ys.astype(F32) * row_w[:, None])
    return out.astype(x.dtype).reshape(B, S, D)


def setup_inputs(seed: int = 0) -> dict:
    key = jax.random.key(seed)
    ks = jax.random.split(key, 24)
    nrm = lambda k, shape, s: jax.random.normal(k, shape, F32) * s
    L, D, dh = DEPTH, D_MODEL, ATT_HEAD_DIM
    return {
        "x": nrm(ks[0], (BATCH, SEQ, D), 1.0),
        "positions": jnp.broadcast_to(jnp.arange(SEQ, dtype=jnp.int32), (BATCH, SEQ)),
        "w_in": nrm(ks[1], (L, D, IN_WIDTH), D ** -0.5),
        "pe_cmp": nrm(ks[2], (L, L_CMP, dh), 0.1),
        "w_ck1": nrm(ks[3], (L, L_CMP * dh, CMP_HIDDEN), (L_CMP * dh) ** -0.5),
        "w_ck2": nrm(ks[4], (L, CMP_HIDDEN, dh), CMP_HIDDEN ** -0.5),
        "w_cv1": nrm(ks[5], (L, L_CMP * dh, CMP_HIDDEN), (L_CMP * dh) ** -0.5),
        "w_cv2": nrm(ks[6], (L, CMP_HIDDEN, dh), CMP_HIDDEN ** -0.5),
        "hg_lb": nrm(ks[7], (L + 1, HG_HEADS * HG_DK), 0.1),
        "hg_norm_g": 1.0 + nrm(ks[8], (L, HG_DV), 0.01),
        "w_o": nrm(ks[9], (L, MIX_WIDTH, D), MIX_WIDTH ** -0.5 * DN_BETA),
        "ln1_g": 1.0 + nrm(ks[10], (L, D), 0.01),
        "ln1_b": nrm(ks[11], (L, D), 0.01),
        "w_router": nrm(ks[12], (L, D, N_EXPERTS), D ** -0.5),
        "b_router": nrm(ks[13], (L, N_EXPERTS), 0.01),
        "w_gate_up": nrm(ks[14], (L, N_EXPERTS, D, 2 * D_FF), D ** -0.5),
        "b_gate_up": nrm(ks[15], (L, N_EXPERTS, 2 * D_FF), 0.01),
        "w_down": nrm(ks[16], (L, N_EXPERTS, D_FF, D), D_FF ** -0.5 * DN_BETA),
        "b_down": nrm(ks[17], (L, N_EXPERTS, D), 0.01),
        "ln2_g": 1.0 + nrm(ks[18], (L, D), 0.01),
        "ln2_b": nrm(ks[19], (L, D), 0.01),
    }


def reference(x, positions, w_in, pe_cmp, w_ck1, w_ck2, w_cv1, w_cv2, hg_lb, hg_norm_g,
              w_o, ln1_g, ln1_b, w_router, b_router, w_gate_up, b_gate_up, w_down, b_down,
              ln2_g, ln2_b):
    lbs = jnp.cumsum(jax.nn.softmax(hg_lb.astype(F32), axis=0), axis=0)
    split_at = np.cumsum(IN_SIZES)[:-1].tolist()
    h = x
    for l in range(DEPTH):
        proj = h @ w_in[l]
        (q_a, k_c, v_c, k_s, v_s, k_w, v_w, g_a, q_h, f_h, i_h, g_h) = jnp.split(proj, split_at, axis=-1)
        att = nsa_attention(q_a, k_c, v_c, k_s, v_s, k_w, v_w, g_a, positions,
                            pe_cmp[l], w_ck1[l], w_ck2[l], w_cv1[l], w_cv2[l])
        rec = hgrn2(q_h, f_h, i_h, g_h, lbs[l], hg_norm_g[l])
        mix = jnp.concatenate([att, rec], axis=-1) @ w_o[l]
        h = layer_norm(DN_ALPHA * h + mix, ln1_g[l], ln1_b[l])
        ffn = moe(h, w_router[l], b_router[l], w_gate_up[l], b_gate_up[l], w_down[l], b_down[l])
        h = layer_norm(DN_ALPHA * h + ffn, ln2_g[l], ln2_b[l])
    return h
```

```python
import numpy as np
import ml_dtypes
import concourse.bass as bass
import concourse.mybir as mybir
from concourse.bass_utils import run_bass_kernel_spmd

F32 = mybir.dt.float32
BF16 = mybir.dt.bfloat16
I32 = mybir.dt.int32
ALU = mybir.AluOpType
AF = mybir.ActivationFunctionType
AX = mybir.AxisListType

S = 8192
D = 1024
NQB = 64
NI = 32
NEG = -30000.0
ALPHA = 2.0 ** 0.25
EPOCH = 16000
DEPOCH = 1800

DEBUG = {}


class Buf:
    __slots__ = ("lw", "rd", "name")

    def __init__(self, name=""):
        self.lw = None
        self.rd = {}
        self.name = name


class T:
    def __init__(self, ap, name="", psum=False):
        self.ap = ap
        self.b = Buf(name)
        self.psum = psum

    def __getitem__(self, k):
        return self.ap[k]


class Sched:
    ENGS = ["pe", "act", "dve", "pool", "sp"]

    def __init__(self, nc):
        self.nc = nc
        self.ops = {e: [] for e in self.ENGS}
        self.cnt = {(e, k): 0 for e in self.ENGS for k in "cd"}
        self.seen = {e: {} for e in self.ENGS}

    def op(self, eng, fn, r=(), w=(), dma=False):
        kind = "d" if dma else "c"
        need = {}

        def add(ident):
            if ident is None:
                return
            key = (ident[0], ident[1])
            if need.get(key, 0) < ident[2]:
                need[key] = ident[2]

        for t in r:
            add(t.b.lw)
            if t.psum:
                for key, c in t.b.rd.items():
                    if key[0] != eng:
                        add((key[0], key[1], c))
        for t in w:
            add(t.b.lw)
            for key, c in t.b.rd.items():
                add((key[0], key[1], c))
        for key, c in need.items():
            if key == ("pe", "c") and eng == "pe" and not dma:
                continue
            if self.seen[eng].get(key, 0) >= c:
                continue
            self.seen[eng][key] = c
            self.ops[eng].append(("w", key, c))
        self.cnt[(eng, kind)] += 1
        c = self.cnt[(eng, kind)]
        self.ops[eng].append(("o", fn, kind, c))
        for t in w:
            t.b.lw = (eng, kind, c)
            t.b.rd = {}
        for t in r:
            if t.b.rd.get((eng, kind), 0) < c:
                t.b.rd[(eng, kind)] = c

    def finish_waits(self, eng):
        for key, c in self.cnt.items():
            if c > 0 and self.seen[eng].get(key, 0) < c and key != (eng, "c"):
                self.seen[eng][key] = c
                self.ops[eng].append(("w", key, c))

    def emit(self, sems):
        nc = self.nc

        def semof(key, c):
            ep = EPOCH if key[1] == "c" else DEPOCH
            e = (c - 1) // ep
            v = (c - 1) % ep + 1
            return sems[(key[0], key[1], e)], (v if key[1] == "c" else 16 * v), e

        with nc.Block() as block:
            def run(engname):
                def body(e):
                    for o in self.ops[engname]:
                        if o[0] == "w":
                            sem, v, ep = semof(o[1], o[2])
                            if o[1][1] == "d" and ep > 0:
                                e.wait_ge(sems[(o[1][0], "d", ep - 1)], 16 * DEPOCH)
                            e.wait_ge(sem, v)
                        else:
                            ins = o[1](e)
                            sem, v, ep = semof((engname, o[2]), o[3])
                            ins.then_inc(sem, 1 if o[2] == "c" else 16)
                return body

            block.tensor(run("pe"))
            block.scalar(run("act"))
            block.vector(run("dve"))
            block.gpsimd(run("pool"))
            block.sync(run("sp"))

    def nsems(self):
        out = []
        for (e, k), c in self.cnt.items():
            ep = EPOCH if k == "c" else DEPOCH
            for i in range((max(c, 1) - 1) // ep + 1):
                out.append((e, k, i))
        return out


def build_program(n_i=NI, n_exp=32, dbg=False):
    nc = bass.Bass("TRN2", target_bir_lowering=False)
    sc = Sched(nc)

    def din(name, shape, dt=F32):
        return nc.dram_tensor(name, list(shape), dt, kind="ExternalInput").ap()

    xT_all = din("xT_all", [8, 128, S])
    xT_own = din("xT_own", [8, 128, NI * 128])
    x_own = din("x_own", [NI * 128, D])
    pos_all = din("pos_all", [128, 64], I32)
    pos_own = din("pos_own", [128, 32], I32)
    pos_cmp = din("pos_cmp", [128, 4], I32)
    w_glob = din("w_glob", [8, 128, 1792])
    w_ownd = din("w_own", [8, 128, 2584])
    w_c1 = din("w_c1", [2, 128, 32 * 256])
    w_c2 = din("w_c2", [2, 128, 2, 64])
    peT = din("peT", [128, 32])
    hg_lb = din("hg_lb", [2, 512])
    hg_ng = din("hg_ng", [1, 128])
    w_o = din("w_o", [8, 128, D])
    lnp = din("lnp", [4, D])
    w_r = din("w_r", [8, 128, 32])
    b_r = din("b_r", [1, 32])
    w_gu = din("w_gu", [n_exp, 8, 128, 2048])
    b_gu = din("b_gu", [n_exp, 128, 16])
    w_dn = din("w_dn", [n_exp, 8, 128, D])
    b_dn = din("b_dn", [n_exp, 1, D])
    c_inv = din("c_inv", [128, 8])
    c_ident = din("c_ident", [128, 128])
    c_tri2 = din("c_tri2", [128, 128])
    c_u2 = din("c_u2", [128, 128])
    c_ebig = din("c_ebig", [128, S])
    c_ovl = din("c_ovl", [128, 4, 128])
    c_diag = din("c_diag", [128, 2, 128])
    c_win = din("c_win", [128, 6, 128])
    c_cmpm = din("c_cmpm", [NI, 128, 4, 128])
    c_imp = din("c_imp", [NI, 128, 2, 128])
    c_h = din("c_h", [128, 1])
    out_d = nc.dram_tensor("out", [NI * 128, D], F32, kind="ExternalOutput").ap()
    hres_d = nc.dram_tensor("hres_scr", [NI * 128, D], F32, kind="Internal").ap()
    hT_d = nc.dram_tensor("hT_scr", [8, 128, NI * 128], BF16, kind="Internal").ap()
    dbg_d = None
    if dbg:
        dbg_d = nc.dram_tensor("dbg", [12, 128, 1024], F32, kind="ExternalOutput").ap()

    state = {"off": 16512, "n": 0}
    LIMIT = 229376 - 128

    def sb(shape, dt, name=None):
        nbytes = int(np.prod(shape[1:])) * (4 if dt in (F32, I32) else 2)
        nbytes = (nbytes + 63) // 64 * 64
        off = state["off"]
        state["off"] += nbytes
        assert state["off"] <= LIMIT, ("SBUF overflow", name, state["off"])
        state["n"] += 1
        nm = f"{name or 't'}_{state['n']}"
        th = nc.alloc_sbuf_tensor_at(nm, list(shape), dt, offset=off)
        return T(th.ap() if hasattr(th, "ap") else th[:], nm)

    pst = [nc.alloc_psum_tensor(f"ps{i}", [128, 512], F32) for i in range(8)]
    psb = [T(p.ap() if hasattr(p, "ap") else p[:], f"ps{i}", psum=True) for i, p in enumerate(pst)]
    psrr = {"i": 0}

    def ps():
        t = psb[psrr["i"] % 6]
        psrr["i"] += 1
        return t

    porr = {"i": 0}

    def ps_acc():
        t = psb[6 + porr["i"] % 2]
        porr["i"] += 1
        return t

    def mm(out, lhsT, rhs, start, stop, r, w):
        sc.op("pe", lambda e: e.matmul(out, lhsT, rhs, start=start, stop=stop), r=r, w=w)

    def tr(out, in_, ident_ap, r, w):
        sc.op("pe", lambda e: e.transpose(out, in_, ident_ap), r=r, w=w)

    def act(out, in_, func, r, w, bias=None, scale=1.0, accum=None, eng="act"):
        kw = {}
        if bias is not None:
            kw["bias"] = bias
        if accum is not None:
            kw["accum_out"] = accum
        sc.op(eng, lambda e: e.activation(out, in_, func, scale=scale, **kw), r=r, w=w)

    def tt(out, a, b_, op, r, w, eng="dve"):
        sc.op(eng, lambda e: e.tensor_tensor(out, a, b_, op), r=r, w=w)

    def ts(out, a, s1, s2, op0, op1, r, w, eng="dve"):
        if op1 is None:
            sc.op(eng, lambda e: e.tensor_scalar(out, a, s1, None, op0), r=r, w=w)
        else:
            sc.op(eng, lambda e: e.tensor_scalar(out, a, s1, s2, op0, op1), r=r, w=w)

    def stt(out, a, scal, b_, op0, op1, r, w, eng="dve"):
        sc.op(eng, lambda e: e.scalar_tensor_tensor(out, a, scal, b_, op0, op1), r=r, w=w)

    def cp(out, in_, r, w, eng="dve"):
        if eng == "act":
            sc.op("act", lambda e: e.copy(out, in_), r=r, w=w)
        else:
            sc.op(eng, lambda e: e.tensor_copy(out, in_), r=r, w=w)

    def memset(t, ap, val, eng="dve"):
        sc.op(eng, lambda e: e.memset(ap, val), w=[t])

    def dma(out, in_, r, w, eng="sp"):
        sc.op(eng, lambda e: e.dma_start(out=out, in_=in_), r=r, w=w, dma=True)

    dram = T(None, "dram_in")
    dscr_h = T(None, "hres")
    dscr_hT = T(None, "hT")
    dout = T(None, "out")
    ddbg = T(None, "dbg")

    def dump(slot, t, ap_f32):
        if dbg:
            dma(dbg_d[slot, :, 0:ap_f32.shape[-1]], ap_f32, r=[t], w=[ddbg], eng="pool")

    ident_f = sb([128, 128], F32, "identf")
    ident_b = sb([128, 128], BF16, "identb")
    tri2_f = sb([128, 128], F32, "tri2f")
    tri2_b = sb([128, 128], BF16, "tri2b")
    u2_f = sb([128, 128], F32, "u2f")
    ones_f = sb([128, 1], F32, "onesf")
    ones_b = sb([1, 128], BF16, "onesb")
    inv_t = sb([128, 8], F32, "inv")
    hcol = sb([128, 1], F32, "hcol")
    dma(ident_f[:], c_ident, [dram], [ident_f])
    dma(tri2_f[:], c_tri2, [dram], [tri2_f])
    dma(u2_f[:], c_u2, [dram], [u2_f])
    dma(inv_t[:], c_inv, [dram], [inv_t])
    dma(hcol[:], c_h, [dram], [hcol])
    cp(ident_b[:], ident_f[:], [ident_f], [ident_b])
    cp(tri2_b[:], tri2_f[:], [tri2_f], [tri2_b])
    memset(ones_f, ones_f[:], 1.0)
    memset(ones_b, ones_b[:], 1.0)

    stage = sb([128, 8, 512], F32, "stage")
    stages = [stage]
    strr = {"i": 0}

    def nstage():
        t = stages[0]
        strr["i"] += 1
        return t

    def load_cast(dst_t, dst_ap, src_ap, shape3):
        st = nstage()
        n = shape3
        dma(st[:, :, 0:n], src_ap.rearrange("k p n -> p k n"), [dram], [st])
        cp(dst_ap, st[:, :, 0:n], [st], [dst_t], eng="pool")

    ksel_d = nc.dram_tensor("ksel_scr", [128, S], BF16, kind="Internal").ap()
    kwin_d = nc.dram_tensor("kwin_scr", [128, S], BF16, kind="Internal").ap()
    vsel_d = nc.dram_tensor("vsel_scr", [128, NQB, 130], BF16, kind="Internal").ap()
    vwin_d = nc.dram_tensor("vwin_scr", [128, NQB, 130], BF16, kind="Internal").ap()
    snap_d = nc.dram_tensor("snap_scr", [128, 128, 512], BF16, kind="Internal").ap()
    rec_d = nc.dram_tensor("rec_scr", [NI * 128, 512], BF16, kind="Internal").ap()
    d_ksel, d_kwin, d_vsel, d_vwin, d_snap, d_rec = (T(None, n) for n in ("dks", "dkw", "dvs", "dvw", "dsn", "drec"))

    def barrier():
        for e in sc.ENGS:
            sc.finish_waits(e)

    lbrow = sb([128, 512], F32, "lbrow")
    omlb = sb([128, 512], F32, "omlb")
    ngrow = sb([128, 128], F32, "ngrow")
    dma(ngrow[:], hg_ng[0:1, :].partition_broadcast(128), [dram], [ngrow])
    lnrow = sb([128, 2, D], F32, "lnrow")
    for k in range(2):
        dma(lnrow[:, k, :], lnp[k:k + 1, :].partition_broadcast(128), [dram], [lnrow])
    brrow = sb([128, 32], F32, "brrow")
    dma(brrow[:], b_r[0:1, :].partition_broadcast(128), [dram], [brrow])
    wr_f = sb([128, 8, 32], F32, "wrf")
    dma(wr_f[:], w_r.rearrange("k p n -> p k n"), [dram], [wr_f])
    gatew = sb([128, NI, 32], F32, "gatew")
    kcT = sb([128, 512], BF16, "kcT")
    Rt = [sb([128, 4, 193], BF16, "R%d" % g) for g in range(2)]
    stat = sb([128, 8], F32, "stat")
    mx8 = sb([128, 16], F32, "mx8")
    rtmp = [sb([128, 8, 32], F32, "rtmp%d" % i) for i in range(2)]
    rrr = {"i": 0}
    fh_ = sb([128, 512], F32, "fh")
    lf_ = sb([128, 512], F32, "lf")
    kk_ = sb([128, 512], F32, "kk")
    mark_persist = state["off"]

    lbraw = sb([128, 2, 512], F32, "lbraw")
    dma(lbraw[:, 0, :], hg_lb[0:1, :].partition_broadcast(128), [dram], [lbraw])
    dma(lbraw[:, 1, :], hg_lb[1:2, :].partition_broadcast(128), [dram], [lbraw])
    tt(lbrow[:], lbraw[:, 0, :], lbraw[:, 1, :], ALU.subtract, [lbraw], [lbrow])
    act(lbrow[:], lbrow[:], AF.Sigmoid, [lbrow], [lbrow])
    ts(omlb[:], lbrow[:], -1.0, 1.0, ALU.mult, ALU.add, [lbrow], [omlb])

    def rope_tables(pos_ap, ntile, name):
        pi_ = sb([128, ntile], I32, name + "pi")
        pf = sb([128, ntile], F32, name + "pf")
        ang = sb([128, ntile, 8], F32, name + "ang")
        cs = sb([128, ntile, 8], F32, name + "cos")
        sn = sb([128, ntile, 8], F32, name + "sin")
        dma(pi_[:], pos_ap, [dram], [pi_])
        cp(pf[:], pi_[:], [pi_], [pf])
        for j in range(ntile):
            ts(ang[:, j, :], inv_t[:], pf[:, j:j + 1], None, ALU.mult, None, [inv_t, pf], [ang])
        two_pi = 2.0 * np.pi
        ki = sb([128, ntile, 8], I32, name + "ki")
        kf = sb([128, ntile, 8], F32, name + "kf")

        def sin_of(dst, shift):
            ts(dst[:], ang[:], shift, None, ALU.add, None, [ang], [dst])
            ts(kf[:], dst[:], 1.0 / two_pi, None, ALU.mult, None, [dst], [kf])
            cp(ki[:], kf[:], [kf], [ki])
            cp(kf[:], ki[:], [ki], [kf])
            stt(dst[:], kf[:], -two_pi, dst[:], ALU.mult, ALU.add, [kf, dst], [dst])
            ts(kf[:], dst[:], float(np.pi), None, ALU.is_gt, None, [dst], [kf])
            stt(dst[:], kf[:], -two_pi, dst[:], ALU.mult, ALU.add, [kf, dst], [dst])
            ts(kf[:], dst[:], -float(np.pi), None, ALU.is_lt, None, [dst], [kf])
            stt(dst[:], kf[:], two_pi, dst[:], ALU.mult, ALU.add, [kf, dst], [dst])
            ts(dst[:], dst[:], float(np.pi), -float(np.pi), ALU.min, ALU.max, [dst], [dst])
            act(dst[:], dst[:], AF.Sin, [dst], [dst])

        sin_of(sn, 0.0)
        sin_of(cs, float(np.pi / 2))
        return cs, sn

    def rope(dst_t, dst3, src_t, src3, nh, cs, sn, j):
        tmp = rtmp[rrr["i"] % 2]
        rrr["i"] += 1
        c_b = cs[:, j, :].unsqueeze(1).broadcast_to([128, nh, 8])
        s_b = sn[:, j, :].unsqueeze(1).broadcast_to([128, nh, 8])
        x1 = src3[:, :, 0:8]
        x2 = src3[:, :, 8:16]
        tt(tmp[:, 0:nh, 0:8], x1, c_b, ALU.mult, [src_t, cs], [tmp])
        tt(tmp[:, 0:nh, 8:16], x2, s_b, ALU.mult, [src_t, sn], [tmp])
        tt(tmp[:, 0:nh, 16:24], x2, c_b, ALU.mult, [src_t, cs], [tmp])
        tt(tmp[:, 0:nh, 24:32], x1, s_b, ALU.mult, [src_t, sn], [tmp])
        tt(dst3[:, :, 0:8], tmp[:, 0:nh, 0:8], tmp[:, 0:nh, 8:16], ALU.subtract, [tmp], [dst_t])
        tt(dst3[:, :, 8:16], tmp[:, 0:nh, 16:24], tmp[:, 0:nh, 24:32], ALU.add, [tmp], [dst_t])
        cp(dst3[:, :, 16:64], src3[:, :, 16:64], [src_t], [dst_t], eng="act")

    def hg_gates(zf_ps, zf_ap):
        act(fh_[:], zf_ap, AF.Sigmoid, [zf_ps], [fh_])
        tt(kk_[:], fh_[:], omlb[:], ALU.mult, [fh_, omlb], [kk_])
        tt(lf_[:], kk_[:], lbrow[:], ALU.add, [kk_, lbrow], [lf_])
        act(lf_[:], lf_[:], AF.Ln, [lf_], [lf_])
        tt(kk_[:], omlb[:], kk_[:], ALU.subtract, [kk_, omlb], [kk_])
        return lf_, kk_

    def finish():
        sc.finish_waits("sp")
        keys = sc.nsems()
        import contextlib
        with contextlib.ExitStack() as es:
            sems = {}
            for (e, k, ep) in keys:
                sems[(e, k, ep)] = es.enter_context(nc.semaphore(f"s_{e}_{k}_{ep}"))
            sc.emit(sems)
        return nc

    import os as _os
    STOP = _os.environ.get("KSTOP", "")
    wg = sb([128, 8, 1792], BF16, "wg")
    for c0 in range(0, 1792, 512):
        n = min(512, 1792 - c0)
        load_cast(wg, wg[:, :, c0:c0 + n], w_glob[:, :, c0:c0 + n], n)
    kcmpT = sb([128, S + 32], BF16, "kcmpT")
    vcmpT = sb([128, S + 32], BF16, "vcmpT")
    memset(kcmpT, kcmpT[:, S:S + 32], 0.0)
    memset(vcmpT, vcmpT[:, S:S + 32], 0.0)
    mark_1a = state["off"]
    cos_a, sin_a = rope_tables(pos_all, 64, "ra")
    hstate = sb([128, 4, 128], F32, "hstate")
    memset(hstate, hstate[:], 0.0)
    xg_p = [sb([128, 8, 512], BF16, "xg%d" % i) for i in range(2)]
    ktm = sb([128, 2, 128], BF16, "ktm")
    kst_p = [sb([128, 2, 512], BF16, "kst%d" % i) for i in range(2)]
    vst_p = [sb([128, 2, 4, 130], BF16, "vst%d" % i) for i in range(2)]
    snp_p = [sb([128, 8, 512], BF16, "snp%d" % i) for i in range(2)]
    ex_ = sb([128, 512], F32, "ex")
    kd_ = sb([128, 512], BF16, "kd")
    vv_ = sb([128, 512], BF16, "vv")
    dec_ = sb([128, 8], F32, "dec")
    for t_ in vst_p:
        memset(t_, t_[:].rearrange("p b t (g d) -> p (b t g) d", d=65)[:, :, 64:65], 1.0)

    def global_group(tg):
        st = nstage()
        xg = xg_p[tg % 2]
        kst = kst_p[tg % 2]
        vst = vst_p[tg % 2]
        snp = snp_p[tg % 2]
        dma(st[:], xT_all[:, :, tg * 512:(tg + 1) * 512].rearrange("k p n -> p k n"), [dram], [st])
        cp(xg[:], st[:], [st], [xg], eng="pool")
        for tl in range(4):
            gt = tg * 4 + tl
            xs = xg[:, :, tl * 128:(tl + 1) * 128]
            p1 = ps()
            for kc in range(8):
                mm(p1[:, 0:512], xs[:, kc, :], wg[:, kc, 0:512], kc == 0, kc == 7, [xg, wg], [p1])
            rope(ktm, ktm[:, :, :].rearrange("p a (g d) -> p (a g) d", g=2),
                 p1, p1[:, 0:256].rearrange("p (a d) -> p a d", d=64), 4, cos_a, sin_a, gt)
            for br in range(2):
                cp(vst[:, br, tl, :].rearrange("p (g d) -> p g d", d=65)[:, :, 0:64],
                   p1[:, 256 + br * 128:384 + br * 128].rearrange("p (g d) -> p g d", g=2), [p1], [vst], eng="act")
            p2 = ps()
            p2b = p2.ap.bitcast(BF16)
            tr(p2b[:, 0:128], ktm[:, 0, :], ident_b[:], [ktm, ident_b], [p2])
            tr(p2b[:, 128:256], ktm[:, 1, :], ident_b[:], [ktm, ident_b], [p2])
            cp(kst[:, 0, tl * 128:(tl + 1) * 128], p2b[:, 0:128], [p2], [kst])
            cp(kst[:, 1, tl * 128:(tl + 1) * 128], p2b[:, 128:256], [p2], [kst])
            p3 = ps()
            for kc in range(8):
                mm(p3[:, 0:128], wg[:, kc, 512:640], xs[:, kc, :], kc == 0, kc == 7, [xg, wg], [p3])
            for kc in range(8):
                mm(p3[:, 128:256], wg[:, kc, 640:768], xs[:, kc, :], kc == 0, kc == 7, [xg, wg], [p3])
            cp(kcmpT[:, gt * 128:(gt + 1) * 128], p3[:, 0:128], [p3], [kcmpT], eng="act")
            cp(vcmpT[:, gt * 128:(gt + 1) * 128], p3[:, 128:256], [p3], [vcmpT], eng="act")
            pf_ = ps()
            pi2 = ps()
            for kc in range(8):
                mm(pf_[:, :], xs[:, kc, :], wg[:, kc, 768:1280], kc == 0, kc == 7, [xg, wg], [pf_])
            for kc in range(8):
                mm(pi2[:, :], xs[:, kc, :], wg[:, kc, 1280:1792], kc == 0, kc == 7, [xg, wg], [pi2])
            lf, kk = hg_gates(pf_, pf_[:, :])
            cp(vv_[:], pi2[:, :], [pi2], [vv_], eng="act")
            pu = ps()
            mm(pu[:, :], u2_f[:], lf[:], True, True, [u2_f, lf], [pu])
            act(ex_[:], pu[:, :], AF.Exp, [pu], [ex_])
            tt(kd_[:], kk[:], ex_[:], ALU.mult, [kk, ex_], [kd_])
            pt = ps()
            for ch in range(2):
                for hd in range(4):
                    mm(pt[:, ch * 4 + hd:ch * 4 + hd + 1], lf[ch * 64:(ch + 1) * 64, hd * 128:(hd + 1) * 128],
                       ones_f[ch * 64:(ch + 1) * 64, 0:1], True, True, [lf, ones_f], [pt])
            act(dec_[:], pt[:, 0:8], AF.Exp, [pt], [dec_])
            for ch in range(2):
                cl = tl * 2 + ch
                cp(snp[:, cl, :], hstate[:].rearrange("p h d -> p (h d)"), [hstate], [snp], eng="act")
                pS = ps()
                for hd in range(4):
                    mm(pS[:, hd * 128:(hd + 1) * 128], kd_[ch * 64:(ch + 1) * 64, hd * 128:(hd + 1) * 128],
                       vv_[ch * 64:(ch + 1) * 64, hd * 128:(hd + 1) * 128], True, True, [kd_, vv_], [pS])
                tt(hstate[:], hstate[:], dec_[:, ch * 4:(ch + 1) * 4].unsqueeze(2).broadcast_to([128, 4, 128]),
                   ALU.mult, [hstate, dec_], [hstate])
                tt(hstate[:], hstate[:], pS[:, :].rearrange("p (h d) -> p h d", h=4), ALU.add, [hstate, pS], [hstate])
        tok = slice(tg * 512, (tg + 1) * 512)
        dma(ksel_d[:, tok], kst[:, 0, :], [kst], [d_ksel], eng="pool")
        dma(kwin_d[:, tok], kst[:, 1, :], [kst], [d_kwin], eng="pool")
        dma(vsel_d[:, tg * 4:(tg + 1) * 4, :], vst[:, 0, :, :], [vst], [d_vsel], eng="pool")
        dma(vwin_d[:, tg * 4:(tg + 1) * 4, :], vst[:, 1, :, :], [vst], [d_vwin], eng="pool")
        dma(snap_d[:, tg * 8:(tg + 1) * 8, :], snp[:], [snp], [d_snap], eng="pool")

    for tg in range(16):
        global_group(tg)

    if STOP == "1a":
        return finish()
    barrier()
    state["off"] = mark_1a
    cos_c, sin_c = rope_tables(pos_cmp, 4, "rc")
    w1 = sb([128, 32, 256], BF16, "w1")
    w2p = sb([128, 2, 2, 128], BF16, "w2p")
    w2f = sb([128, 2, 64], F32, "w2f")
    pe_f = sb([128, 32], F32, "pef")
    pe_b = sb([128, 32], BF16, "peb")
    bias1 = sb([128, 2], F32, "bias1")
    gl = [sb([128, 2, 512], BF16, "gl%d" % g) for g in range(2)]
    xs_ = sb([128, 512], F32, "cxs")
    x2_ = sb([128, 512], F32, "cx2")
    kct = sb([128, 128], BF16, "kct")
    ovst = sb([128, 4, 128], F32, "ovst")
    dma(pe_f[:], peT, [dram], [pe_f])
    cp(pe_b[:], pe_f[:], [pe_f], [pe_b])
    dma(ovst[:], c_ovl, [dram], [ovst])
    for g in range(2):
        cp(Rt[g][:, :, 65:193], ovst[:], [ovst], [Rt[g]])
        memset(Rt[g], Rt[g][:, :, 64:65], 1.0)
    for kv in range(2):
        srcT = kcmpT if kv == 0 else vcmpT
        for c0 in range(0, 32 * 256, 4096):
            st = nstage()
            dma(st[:].rearrange("p a b -> p (a b)"), w_c1[kv, :, c0:c0 + 4096], [dram], [st])
            cp(w1[:].rearrange("p a b -> p (a b)")[:, c0:c0 + 4096], st[:].rearrange("p a b -> p (a b)"), [st], [w1], eng="pool")
        dma(w2f[:], w_c2[kv], [dram], [w2f])
        memset(w2p, w2p[:], 0.0)
        for g in range(2):
            cp(w2p[:, :, g, g * 64:(g + 1) * 64], w2f[:], [w2f], [w2p])
        for g in range(2):
            rows = slice(g * 64, (g + 1) * 64)
            for hc in range(2):
                pb = ps()
                for l in range(32):
                    mm(pb[:, 0:1], w1[rows, l, hc * 128:(hc + 1) * 128], pe_b[rows, l:l + 1], l == 0, l == 31, [w1, pe_b], [pb])
                cp(bias1[:, hc:hc + 1], pb[:, 0:1], [pb], [bias1])
                ph = ps()
                for l in range(32):
                    rhs = srcT[rows, l:l + 16 * 512].rearrange("p (c s) -> p c s", s=16)[:, :, 0]
                    mm(ph[:, :], w1[rows, l, hc * 128:(hc + 1) * 128], rhs, l == 0, l == 31, [w1, srcT], [ph])
                act(xs_[:], ph[:, :], AF.Identity, [ph, bias1], [xs_], bias=bias1[:, hc:hc + 1])
                tt(x2_[:], xs_[:], xs_[:], ALU.mult, [xs_], [x2_])
                ts(x2_[:], x2_[:], 0.044715, 1.0, ALU.mult, ALU.add, [x2_], [x2_])
                tt(x2_[:], x2_[:], xs_[:], ALU.mult, [x2_, xs_], [x2_])
                act(x2_[:], x2_[:], AF.Sigmoid, [x2_], [x2_], scale=1.5957691216057308)
                tt(gl[g][:, hc, :], xs_[:], x2_[:], ALU.mult, [xs_, x2_], [gl[g]])
        for cc in range(4):
            po = ps()
            n = 0
            for g in range(2):
                for hc in range(2):
                    mm(po[:, 0:128], gl[g][:, hc, cc * 128:(cc + 1) * 128], w2p[:, hc, g, :], n == 0, n == 3, [gl[g], w2p], [po])
                    n += 1
            if kv == 0:
                rope(kct, kct[:, :].rearrange("p (g d) -> p g d", g=2), po,
                     po[:, 0:128].rearrange("p (g d) -> p g d", g=2), 2, cos_c, sin_c, cc)
                p2 = ps()
                p2b = p2.ap.bitcast(BF16)
                tr(p2b[:, 0:128], kct[:], ident_b[:], [kct, ident_b], [p2])
                cp(kcT[:, cc * 128:(cc + 1) * 128], p2b[:, 0:128], [p2], [kcT])
            else:
                for g in range(2):
                    cp(Rt[g][:, cc, 0:64], po[:, g * 64:(g + 1) * 64], [po], [Rt[g]])

    if STOP == "1b":
        return finish()
    barrier()
    state["off"] = mark_persist
    wo_ = sb([128, 8, 2048], BF16, "wownh")
    for c0 in range(0, 2048, 512):
        load_cast(wo_, wo_[:, :, c0:c0 + 512], w_ownd[:, :, 536 + c0:536 + c0 + 512], 512)
    xo_p = [sb([128, 8, 128], BF16, "xo%d" % i) for i in range(2)]
    eb_ = sb([128, 512], F32, "eb")
    qe_ = sb([128, 512], BF16, "qe")
    ke_ = sb([128, 512], BF16, "ke")
    qeT = sb([128, 4, 128], BF16, "qeT")
    keT = sb([128, 4, 128], BF16, "keT")
    AT_ = sb([128, 4, 128], BF16, "AT")
    vo_ = sb([128, 512], BF16, "vo")
    snl = sb([128, 4, 512], BF16, "snl")
    sel0 = sb([128, 4, 128], BF16, "sel0")
    sel1 = sb([128, 4, 128], BF16, "sel1")
    sdf = sb([128, 512], F32, "sdf")
    orec = sb([128, 4, 128], F32, "orec")
    sq_ = sb([128, 128], F32, "sq")
    gsl = sb([128, 512], F32, "gsl")
    cat_h = sb([128, 512], BF16, "cath")

    def own_hgrn(i):
        xo = xo_p[i % 2]
        st = nstage()
        dma(st[:, :, 0:128], xT_own[:, :, i * 128:(i + 1) * 128].rearrange("k p n -> p k n"), [dram], [st])
        cp(xo[:], st[:, :, 0:128], [st], [xo], eng="pool")
        dma(snl[:], snap_d[:, 4 * i:4 * i + 4, :], [d_snap], [snl], eng="pool")

        def proj(c0, n):
            p = ps()
            for kc in range(8):
                mm(p[:, 0:n], xo[:, kc, :], wo_[:, kc, c0:c0 + n], kc == 0, kc == 7, [xo, wo_], [p])
            return p

        pzq = proj(0, 512)
        pzf = proj(512, 512)
        lf, kk = hg_gates(pzf, pzf[:, :])
        pb_ = ps()
        mm(pb_[:, :], tri2_f[:], lf[:], True, True, [tri2_f, lf], [pb_])
        act(eb_[:], pb_[:, :], AF.Exp, [pb_], [eb_])
        tt(qe_[:], pzq[:, :], eb_[:], ALU.mult, [pzq, eb_], [qe_])
        act(eb_[:], pb_[:, :], AF.Exp, [pb_], [eb_], scale=-1.0)
        tt(ke_[:], kk[:], eb_[:], ALU.mult, [kk, eb_], [ke_])
        pzi = proj(1024, 512)
        cp(vo_[:], pzi[:, :], [pzi], [vo_], eng="act")
        pt1 = ps()
        pt1b = pt1.ap.bitcast(BF16)
        for hd in range(4):
            tr(pt1b[:, hd * 128:(hd + 1) * 128], qe_[:, hd * 128:(hd + 1) * 128], ident_b[:], [qe_, ident_b], [pt1])
        cp(qeT[:].rearrange("p h t -> p (h t)"), pt1b[:, 0:512], [pt1], [qeT])
        pt2 = ps()
        pt2b = pt2.ap.bitcast(BF16)
        for hd in range(4):
            tr(pt2b[:, hd * 128:(hd + 1) * 128], ke_[:, hd * 128:(hd + 1) * 128], ident_b[:], [ke_, ident_b], [pt2])
        cp(keT[:].rearrange("p h t -> p (h t)"), pt2b[:, 0:512], [pt2], [keT], eng="act")
        pA = ps()
        for hd in range(4):
            mm(pA[:, hd * 128:(hd + 1) * 128], keT[:, hd, :], qeT[:, hd, :], True, True, [keT, qeT], [pA])
        tt(AT_[:], pA[:, :].rearrange("p (h t) -> p h t", h=4), tri2_b[:].unsqueeze(1).broadcast_to([128, 4, 128]),
           ALU.mult, [pA, tri2_b], [AT_])
        for (sel, a, b_) in ((sel0, 0, 2), (sel1, 1, 3)):
            tt(sdf[:], snl[:, b_, :], snl[:, a, :], ALU.subtract, [snl], [sdf])
            stt(sel[:].rearrange("p h d -> p (h d)"), sdf[:], hcol[:, 0:1], snl[:, a, :], ALU.mult, ALU.add, [sdf, hcol, snl], [sel])
        po_ = ps()
        for hd in range(4):
            mm(po_[:, hd * 128:(hd + 1) * 128], AT_[:, hd, :], vo_[:, hd * 128:(hd + 1) * 128], True, False, [AT_, vo_], [po_])
            mm(po_[0:64, hd * 128:(hd + 1) * 128], qeT[:, hd, 0:64], sel0[:, hd, :], False, False, [qeT, sel0], [po_])
            mm(po_[64:128, hd * 128:(hd + 1) * 128], qeT[:, hd, 64:128], sel1[:, hd, :], False, True, [qeT, sel1], [po_])
        cp(orec[:].rearrange("p h d -> p (h d)"), po_[:, :], [po_], [orec], eng="act")
        for hd in range(4):
            act(sq_[:], orec[:, hd, :], AF.Square, [orec], [sq_, stat], accum=stat[:, 4 + hd:5 + hd])
        ts(stat[:, 4:8], stat[:, 4:8], 1.0 / 128, 1e-6, ALU.mult, ALU.add, [stat], [stat])
        act(stat[:, 4:8], stat[:, 4:8], AF.Ln, [stat], [stat])
        act(stat[:, 4:8], stat[:, 4:8], AF.Exp, [stat], [stat], scale=-0.5)
        pzg = proj(1536, 512)
        act(gsl[:], pzg[:, :], AF.Silu, [pzg], [gsl])
        for hd in range(4):
            stt(orec[:, hd, :], orec[:, hd, :], stat[:, 4 + hd:5 + hd], ngrow[:], ALU.mult, ALU.mult, [orec, stat, ngrow], [orec])
        tt(cat_h[:], orec[:].rearrange("p h d -> p (h d)"), gsl[:], ALU.mult, [orec, gsl], [cat_h])
        if dbg and i == DEBUG.get("i", 0):
            dump(1, orec, orec[:].rearrange("p h d -> p (h d)"))

        dma(rec_d[i * 128:(i + 1) * 128, :], cat_h[:], [cat_h], [d_rec], eng="pool")

    for i in range(n_i):
        own_hgrn(i)

    if STOP == "1cA":
        return finish()
    barrier()
    state["off"] = mark_persist
    wo_ = sb([128, 8, 536], BF16, "wownq")
    load_cast(wo_, wo_[:, :, 0:512], w_ownd[:, :, 0:512], 512)
    load_cast(wo_, wo_[:, :, 512:536], w_ownd[:, :, 512:536], 24)
    wob = sb([128, 8, D], BF16, "wob")
    for c0 in range(0, D, 512):
        load_cast(wob, wob[:, :, c0:c0 + 512], w_o[:, :, c0:c0 + 512], 512)
    ebig = sb([128, S], BF16, "ebig")
    for c0 in range(0, S, 4096):
        st = nstage()
        dma(st[:].rearrange("p a b -> p (a b)"), c_ebig[:, c0:c0 + 4096], [dram], [st])
        cp(ebig[:, c0:c0 + 4096], st[:].rearrange("p a b -> p (a b)"), [st], [ebig], eng="pool")
    cos_o, sin_o = rope_tables(pos_own, 32, "ro")
    xo_p = [sb([128, 8, 128], BF16, "xo%d" % i) for i in range(2)]
    qT = sb([128, 4, 128], BF16, "qT")
    qtm = sb([128, 8, 64], BF16, "qtm")
    gat = sb([128, 24], F32, "gat")
    pT_p = [sb([128, 512], BF16, "pT%d" % i) for i in range(3)]
    prr = {"i": 0}
    cmpm_f = sb([128, 4, 128], F32, "cmpmf")
    cm = sb([128, 4, 128], BF16, "cmpm")
    ic = sb([128, 2, 128], F32, "impc")
    diag_f = sb([128, 2, 128], F32, "diagf")
    diag_b = sb([128, 2, 4, 128], BF16, "diagb")
    win_f = sb([128, 6, 128], F32, "winf")
    win_b = sb([128, 6, 4, 128], BF16, "winb")
    dma(diag_f[:], c_diag, [dram], [diag_f])
    dma(win_f[:], c_win, [dram], [win_f])
    for n in range(4):
        cp(diag_b[:, :, n, :], diag_f[:], [diag_f], [diag_b])
        cp(win_b[:, :, n, :], win_f[:], [win_f], [win_b])
    zb = sb([128, 512], BF16, "zb")
    memset(zb, zb[:], 0.0)
    oc_s = sb([128, 4, 193], F32, "ocs")
    rs_s = sb([128, 4, 4], F32, "rss")
    imp = sb([128, 128], F32, "imp")
    imp2 = sb([128, 128], F32, "imp2")
    selm = sb([128, 128], BF16, "selm")
    negm = sb([128, 4, 128], BF16, "negm")
    oacc = sb([128, 2, 4, 64], F32, "oacc")
    otmp = sb([128, 4, 64], F32, "otmp")
    cat_b = sb([128, D], BF16, "catb")
    catT = sb([128, 8, 128], BF16, "catT")
    xres = sb([128, D], F32, "xres")
    r1 = sb([128, D], F32, "r1")
    sq_big = sb([128, D], BF16, "sqbig")
    hT_f = sb([128, 8, 128], F32, "hTf")
    hT_b = sb([128, 8, 128], BF16, "hTb")
    lg = sb([128, 32], F32, "lg")
    lg2 = sb([128, 32], F32, "lg2")
    kch_p = [sb([128, 1024], BF16, "kch%d" % i) for i in range(2)]
    vch_p = [sb([128, 8, 130], BF16, "vch%d" % i) for i in range(2)]
    chr_ = {"i": 0}

    def layer_norm(dst_t, dst_ap, src_t, src_ap, grow, brow, lnt):
        sc.op("dve", lambda e: e.reduce_sum(stat[:, 0:1], src_ap, AX.X), r=[src_t], w=[stat])
        ts(stat[:, 1:2], stat[:, 0:1], -1.0 / D, None, ALU.mult, None, [stat], [stat])
        act(dst_ap, src_ap, AF.Identity, [src_t, stat], [dst_t], bias=stat[:, 1:2])
        act(sq_big[:], dst_ap, AF.Square, [dst_t], [sq_big, stat], accum=stat[:, 2:3])
        ts(stat[:, 3:4], stat[:, 2:3], 1.0 / D, 1e-5, ALU.mult, ALU.add, [stat], [stat])
        act(stat[:, 3:4], stat[:, 3:4], AF.Ln, [stat], [stat])
        act(stat[:, 3:4], stat[:, 3:4], AF.Exp, [stat], [stat], scale=-0.5)
        stt(dst_ap, dst_ap, stat[:, 3:4], grow, ALU.mult, ALU.mult, [dst_t, stat, lnt], [dst_t])
        tt(dst_ap, dst_ap, brow, ALU.add, [dst_t, lnt], [dst_t])

    def own_block(i):
        xo = xo_p[i % 2]
        st = nstage()
        dma(st[:, :, 0:128], xT_own[:, :, i * 128:(i + 1) * 128].rearrange("k p n -> p k n"), [dram], [st])
        cp(xo[:], st[:, :, 0:128], [st], [xo], eng="pool")
        dma(xres[:], x_own[i * 128:(i + 1) * 128, :], [dram], [xres], eng="pool")
        dma(cmpm_f[:], c_cmpm[i], [dram], [cmpm_f], eng="pool")
        cp(cm[:], cmpm_f[:], [cmpm_f], [cm])
        dma(ic[:], c_imp[i], [dram], [ic], eng="pool")

        def proj(c0, n):
            p = ps()
            for kc in range(8):
                mm(p[:, 0:n], xo[:, kc, :], wo_[:, kc, c0:c0 + n], kc == 0, kc == 7, [xo, wo_], [p])
            return p

        pq = proj(0, 512)
        rope(qtm, qtm[:], pq, pq[:, 0:512].rearrange("p (a d) -> p a d", d=64), 8, cos_o, sin_o, i)
        p2 = ps()
        p2b = p2.ap.bitcast(BF16)
        for n in range(4):
            tr(p2b[:, n * 128:(n + 1) * 128], qtm[:, 2 * n:2 * n + 2, :].rearrange("p a d -> p (a d)"), ident_b[:], [qtm, ident_b], [p2])
        cp(qT[:].rearrange("p n t -> p (n t)"), p2b[:, 0:512], [p2], [qT])
        pg = proj(512, 24)
        act(gat[:], pg[:, 0:24], AF.Sigmoid, [pg], [gat])

        dma(cat_b[:, 512:1024], rec_d[i * 128:(i + 1) * 128, :], [d_rec], [cat_b], eng="pool")
        if STOP == "B1":
            return
        qb_max = 2 * i + 1
        ncc = (128 * qb_max + 96) // 2048 + 1
        for g in range(2):
            rows = slice(g * 64, (g + 1) * 64)
            qrhs = qT[rows, :, :].rearrange("p n t -> p (n t)")
            pc0 = ps()
            pc1 = ps()
            pcs = [pc0, pc1]
            for pc in pcs:
                mm(pc[:, 0:386], zb[:, 0:128], zb[:, 0:386], True, False, [zb], [pc])
            for cc in range(ncc):
                pS = ps()
                mm(pS[:, :], kcT[rows, cc * 128:(cc + 1) * 128], qrhs, True, True, [kcT, qT], [pS])
                pT = pT_p[prr["i"] % 3]
                prr["i"] += 1
                act(pT[:], pS[:, :], AF.Exp, [pS], [pT], scale=0.125)
                tt(pT[:].rearrange("p (n t) -> p n t", n=4), pT[:].rearrange("p (n t) -> p n t", n=4),
                   cm[:, cc, :].unsqueeze(1).broadcast_to([128, 4, 128]), ALU.mult, [pT, cm], [pT])
                for n in range(4):
                    pc = pcs[n // 2]
                    mm(pc[:, (n % 2) * 193:(n % 2) * 193 + 193], pT[:, n * 128:(n + 1) * 128], Rt[g][:, cc, :],
                       False, cc == ncc - 1, [pT, Rt[g]], [pc])
            for n in range(4):
                cp(oc_s[:, n, :], pcs[n // 2][:, (n % 2) * 193:(n % 2) * 193 + 193], [pcs[n // 2]], [oc_s], eng="act")
            ts(rs_s[:, 0, :], oc_s[:, :, 64], 1e-20, None, ALU.max, None, [oc_s], [rs_s])
            sc.op("dve", lambda e: e.reciprocal(rs_s[:, 0, :], rs_s[:, 0, :]), r=[rs_s], w=[rs_s])
            ts(imp[:], oc_s[:, 0, 65:193], rs_s[:, 0, 0:1], None, ALU.mult, None, [oc_s, rs_s], [imp])
            for n in range(1, 4):
                stt(imp[:], oc_s[:, n, 65:193], rs_s[:, 0, n:n + 1], imp[:], ALU.mult, ALU.add, [oc_s, rs_s, imp], [imp])
            tt(imp[:], imp[:], ic[:, 0, :], ALU.mult, [imp, ic], [imp])
            tt(imp[:], imp[:], ic[:, 1, :], ALU.add, [imp, ic], [imp])
            if STOP == "B2":
                continue
            sc.op("dve", lambda e: e.max(out=mx8[:, 0:8], in_=imp[:]), r=[imp], w=[mx8])
            sc.op("dve", lambda e: e.match_replace(out=imp2[:], in_to_replace=mx8[:, 0:8], in_values=imp[:], imm_value=-1e9),
                  r=[imp, mx8], w=[imp2])
            sc.op("dve", lambda e: e.max(out=mx8[:, 8:16], in_=imp2[:]), r=[imp2], w=[mx8])
            ts(imp2[:], imp[:], mx8[:, 15:16], None, ALU.is_ge, None, [imp, mx8], [imp2])
            tt(selm[:], imp2[:], ic[:, 0, :], ALU.mult, [imp2, ic], [selm])
            if dbg and i == DEBUG.get("i", 0) and g == 0:
                dump(2, imp, imp[:])
                dump(3, imp2, imp2[:])
            pm = ps()
            pmb = pm.ap.bitcast(BF16)
            tr(pmb[:, 0:128], selm[:], ident_b[:], [selm, ident_b], [pm])
            ts(negm[:], pmb[:, 0:128].unsqueeze(1).broadcast_to([128, 4, 128]), -1.0, -NEG, ALU.add, ALU.mult, [pm], [negm])
            gv = gat[:, g * 12:(g + 1) * 12].rearrange("p (n b) -> p n b", b=3)
            tt(rs_s[:, 1, :], rs_s[:, 0, :], gv[:, :, 0], ALU.mult, [rs_s, gat], [rs_s])
            tt(oacc[:, g, :, :], oc_s[:, :, 0:64], rs_s[:, 1, :].unsqueeze(2).broadcast_to([128, 4, 64]), ALU.mult, [oc_s, rs_s], [oacc])
            if dbg and i == DEBUG.get("i", 0) and g == 0:
                dump(5, oacc, oacc[:, 0, :, :].rearrange("p n d -> p (n d)"))

            if STOP == "B3":
                continue
            def attn_branch(k_d, v_d, dk_t, dv_t, tiles, extra_mask, bidx):
                po = ps_acc()
                nt = len(tiles)
                mm(po[:, 0:260], zb[:, 0:128], zb[:, 0:260], True, False, [zb], [po])
                kt0 = tiles[0][0]
                kch = vch = None
                for ti, (kt, r_) in enumerate(tiles):
                    if (kt - kt0) % 8 == 0:
                        nk = min(8, tiles[-1][0] + 1 - kt)
                        kch = kch_p[chr_["i"] % 2]
                        vch = vch_p[chr_["i"] % 2]
                        chr_["i"] += 1
                        dma(kch[:, 0:nk * 128], k_d[:, kt * 128:(kt + nk) * 128], [dk_t], [kch])
                        dma(vch[:, 0:nk, :], v_d[:, kt:kt + nk, :], [dv_t], [vch])
                    kl = (kt - kt0) % 8
                    pS = ps()
                    mm(pS[:, :], kch[rows, kl * 128:(kl + 1) * 128], qrhs, True, False, [kch, qT], [pS])
                    extra_mask(pS, kt, r_)
                    pT = pT_p[prr["i"] % 3]
                    prr["i"] += 1
                    act(pT[:], pS[:, :], AF.Exp, [pS], [pT], scale=0.125)
                    for n in range(4):
                        mm(po[:, n * 65:(n + 1) * 65], pT[:, n * 128:(n + 1) * 128], vch[:, kl, g * 65:(g + 1) * 65],
                           False, ti == nt - 1, [pT, vch], [po])
                pv = po[:, 0:260].rearrange("p (n d) -> p n d", d=65)
                ts(rs_s[:, 2, :], pv[:, :, 64], 1e-20, None, ALU.max, None, [po], [rs_s])
                sc.op("dve", lambda e: e.reciprocal(rs_s[:, 2, :], rs_s[:, 2, :]), r=[rs_s], w=[rs_s])
                tt(rs_s[:, 2, :], rs_s[:, 2, :], gv[:, :, bidx], ALU.mult, [rs_s, gat], [rs_s])
                tt(otmp[:], pv[:, :, 0:64], rs_s[:, 2, :].unsqueeze(2).broadcast_to([128, 4, 64]), ALU.mult, [po, rs_s], [otmp])
                if dbg and i == DEBUG.get("i", 0) and g == 0:
                    dump(6 + bidx, otmp, otmp[:].rearrange("p n d -> p (n d)"))
                tt(oacc[:, g, :, :], oacc[:, g, :, :], otmp[:], ALU.add, [oacc, otmp], [oacc])

            def sel_mask(pS, kt, r_):
                last = r_ is None
                mm(pS[:, :], ebig[:, kt * 128:(kt + 1) * 128], negm[:].rearrange("p n t -> p (n t)"), False, last, [ebig, negm], [pS])
                if not last:
                    mm(pS[:, :], ident_b[:], diag_b[:, r_, :, :].rearrange("p n t -> p (n t)"), False, True, [ident_b, diag_b], [pS])

            tiles = [(kt, (kt - 2 * i) if kt >= 2 * i else None) for kt in range(0, 2 * i + 2)]
            attn_branch(ksel_d, vsel_d, d_ksel, d_vsel, tiles, sel_mask, 1)

            def win_mask(pS, kt, r_):
                mm(pS[:, :], ident_b[:], win_b[:, r_ + 4, :, :].rearrange("p n t -> p (n t)"), False, True, [ident_b, win_b], [pS])

            tiles = [(2 * i + r_, r_) for r_ in range(-4, 2) if 2 * i + r_ >= 0]
            attn_branch(kwin_d, vwin_d, d_kwin, d_vwin, tiles, win_mask, 2)
        if STOP in ("B2", "B3"):
            return
        cp(cat_b[:, 0:512], oacc[:].rearrange("p g n d -> p (g n d)"), [oacc], [cat_b])
        if dbg and i == DEBUG.get("i", 0):
            dump(0, oacc, oacc[:].rearrange("p g n d -> p (g n d)"))

        pt = ps()
        ptx = ps()
        for kc in range(8):
            pp = pt if kc < 4 else ptx
            ppb = pp.ap.bitcast(BF16)
            tr(ppb[:, (kc % 4) * 128:(kc % 4 + 1) * 128], cat_b[:, kc * 128:(kc + 1) * 128], ident_b[:], [cat_b, ident_b], [pp])
        cp(catT[:, 0:4, :].rearrange("p k t -> p (k t)"), pt.ap.bitcast(BF16)[:, 0:512], [pt], [catT])
        cp(catT[:, 4:8, :].rearrange("p k t -> p (k t)"), ptx.ap.bitcast(BF16)[:, 0:512], [ptx], [catT])
        for half in range(2):
            pm_ = ps()
            for kc in range(8):
                mm(pm_[:, :], catT[:, kc, :], wob[:, kc, half * 512:(half + 1) * 512], kc == 0, kc == 7, [catT, wob], [pm_])
            stt(r1[:, half * 512:(half + 1) * 512], xres[:, half * 512:(half + 1) * 512], ALPHA, pm_[:, :], ALU.mult, ALU.add,
                [xres, pm_], [r1])
        if STOP == "B4":
            return
        hh = xres
        layer_norm(hh, hh[:], r1, r1[:], lnrow[:, 0, :], lnrow[:, 1, :], lnrow)
        if dbg and i == DEBUG.get("i", 0):
            dump(4, hh, hh[:])
        if STOP == "B5":
            return
        for half in range(2):
            pt_ = ps()
            for kq in range(4):
                kc = half * 4 + kq
                mm(pt_[:, kq * 128:(kq + 1) * 128], hh[:, kc * 128:(kc + 1) * 128], ident_f[:], True, True, [hh, ident_f], [pt_])
            if STOP == "X1":
                continue
            cp(hT_f[:, half * 4:(half + 1) * 4, :].rearrange("p k t -> p (k t)"), pt_[:, :], [pt_], [hT_f], eng="act")
            if STOP == "X2":
                continue
            cp(hT_b[:, half * 4:(half + 1) * 4, :], hT_f[:, half * 4:(half + 1) * 4, :], [hT_f], [hT_b])
        if STOP not in ("B6a", "X1", "X2"):
            dma(hT_d[:, :, i * 128:(i + 1) * 128].rearrange("k p n -> p k n"), hT_b[:], [hT_b], [dscr_hT], eng="pool")
        else:
            return
        if STOP == "B6":
            return
        pl = ps()
        for kc in range(8):
            mm(pl[:, 0:32], hT_f[:, kc, :], wr_f[:, kc, :], kc == 0, kc == 7, [hT_f, wr_f], [pl])
        tt(lg[:], pl[:, 0:32], brrow[:], ALU.add, [pl, brrow], [lg])
        sc.op("dve", lambda e: e.max(out=mx8[:, 0:8], in_=lg[:]), r=[lg], w=[mx8])
        ts(lg2[:], lg[:], mx8[:, 3:4], None, ALU.is_ge, None, [lg, mx8], [lg2])
        ts(stat[:, 0:1], mx8[:, 0:1], -1.0, None, ALU.mult, None, [mx8], [stat])
        act(lg[:], lg[:], AF.Exp, [lg, stat], [lg], bias=stat[:, 0:1])
        tt(lg[:], lg[:], lg2[:], ALU.mult, [lg, lg2], [lg])
        sc.op("dve", lambda e: e.reduce_sum(stat[:, 1:2], lg[:], AX.X), r=[lg], w=[stat])
        sc.op("dve", lambda e: e.reciprocal(stat[:, 1:2], stat[:, 1:2]), r=[stat], w=[stat])
        ts(gatew[:, i, :], lg[:], stat[:, 1:2], None, ALU.mult, None, [lg, stat], [gatew])
        if STOP == "B7":
            return
        ts(r1[:], hh[:], ALPHA, None, ALU.mult, None, [hh], [r1])
        dma(hres_d[i * 128:(i + 1) * 128, :], r1[:], [r1], [dscr_h], eng="pool")

    for i in range(n_i):
        own_block(i)

    if STOP == "1cB" or STOP.startswith("B"):
        return finish()
    barrier()
    state["off"] = mark_persist
    ln2row = sb([128, 2, D], F32, "ln2row")
    for k in range(2):
        dma(ln2row[:, k, :], lnp[2 + k:3 + k, :].partition_broadcast(128), [dram], [ln2row])
    sq_big = sb([128, D], BF16, "sqbig2")
    otile = sb([128, D], F32, "otile")
    hTp = sb([128, 8, 1024], BF16, "hTp")
    acc = sb([128, 8, D], F32, "acc")
    actT = sb([128, 8, 1024], BF16, "actT")
    wgu_p = [sb([128, 8, 256], BF16, "wgu%d" % i) for i in range(2)]
    wdn_p = [sb([128, 8, D], BF16, "wdn%d" % i) for i in range(2)]
    bgu = sb([128, 16], F32, "bgu")
    bdn_f = sb([1, D], F32, "bdnf")
    bdn_b = sb([1, D], BF16, "bdnb")
    c7 = sb([128, 1], F32, "c7")
    memset(c7, c7[:], 7.0)
    gS_p = [sb([128, 512], F32, "gS%d" % i) for i in range(2)]
    sg_p = [sb([128, 512], F32, "sg%d" % i) for i in range(2)]
    uS_p = [sb([128, 512], F32, "uS%d" % i) for i in range(2)]
    err = {"i": 0}
    n_pass = (n_i + 7) // 8
    for p in range(n_pass):
        nt_ = min(8, n_i - p * 8)
        ntok = nt_ * 128
        dma(hTp[:, :, 0:ntok], hT_d[:, :, p * 1024:p * 1024 + ntok].rearrange("k p n -> p k n"), [dscr_hT], [hTp])
        for tl in range(nt_):
            dma(acc[:, tl, :], hres_d[(p * 8 + tl) * 128:(p * 8 + tl + 1) * 128, :], [dscr_h], [acc])
        for e in range(n_exp):
            dma(bgu[:], b_gu[e], [dram], [bgu], eng="pool")
            dma(bdn_f[:], b_dn[e], [dram], [bdn_f], eng="pool")
            cp(bdn_b[:], bdn_f[:], [bdn_f], [bdn_b])
            for fp in range(8):
                st = nstage()
                wgu = wgu_p[(e * 8 + fp) % 2]
                dma(st[:, :, 0:128], w_gu[e, :, :, fp * 128:(fp + 1) * 128].rearrange("k p n -> p k n"), [dram], [st])
                dma(st[:, :, 128:256], w_gu[e, :, :, 1024 + fp * 128:1024 + (fp + 1) * 128].rearrange("k p n -> p k n"), [dram], [st])
                cp(wgu[:], st[:, :, 0:256], [st], [wgu], eng="pool")
                for t0 in range(0, ntok, 512):
                    nn = min(512, ntok - t0)
                    pg_ = ps()
                    pu_ = ps()
                    for kc in range(8):
                        mm(pg_[:, 0:nn], wgu[:, kc, 0:128], hTp[:, kc, t0:t0 + nn], kc == 0, kc == 7, [wgu, hTp], [pg_])
                    for kc in range(8):
                        mm(pu_[:, 0:nn], wgu[:, kc, 128:256], hTp[:, kc, t0:t0 + nn], kc == 0, kc == 7, [wgu, hTp], [pu_])
                    k_ = err["i"] % 2
                    err["i"] += 1
                    gS, sg, uS = gS_p[k_], sg_p[k_], uS_p[k_]
                    ts(gS[:, 0:nn], pg_[:, 0:nn], bgu[:, fp:fp + 1], c7[:, 0:1], ALU.add, ALU.min, [pg_, bgu, c7], [gS])
                    act(sg[:, 0:nn], gS[:, 0:nn], AF.Sigmoid, [gS], [sg], scale=1.702)
                    ts(uS[:, 0:nn], pu_[:, 0:nn], bgu[:, 8 + fp:9 + fp], c7[:, 0:1], ALU.add, ALU.min, [pu_, bgu, c7], [uS])
                    ts(uS[:, 0:nn], uS[:, 0:nn], -7.0, 1.0, ALU.max, ALU.add, [uS], [uS])
                    tt(sg[:, 0:nn], sg[:, 0:nn], gS[:, 0:nn], ALU.mult, [sg, gS], [sg], eng="pool")
                    tt(actT[:, fp, t0:t0 + nn], sg[:, 0:nn], uS[:, 0:nn], ALU.mult, [sg, uS], [actT], eng="pool")
            wdn = wdn_p[e % 2]
            for half in range(2):
                st = nstage()
                dma(st[:], w_dn[e, :, :, half * 512:(half + 1) * 512].rearrange("k p n -> p k n"), [dram], [st])
                cp(wdn[:, :, half * 512:(half + 1) * 512], st[:], [st], [wdn], eng="pool")
            for tl in range(nt_):
                ti = p * 8 + tl
                for half in range(2):
                    py = ps()
                    for fc in range(8):
                        mm(py[:, :], actT[:, fc, tl * 128:(tl + 1) * 128], wdn[:, fc, half * 512:(half + 1) * 512], fc == 0, False, [actT, wdn], [py])
                    mm(py[:, :], ones_b[0:1, :], bdn_b[0:1, half * 512:(half + 1) * 512], False, True, [ones_b, bdn_b], [py])
                    stt(acc[:, tl, half * 512:(half + 1) * 512], py[:, :], gatew[:, ti, e:e + 1], acc[:, tl, half * 512:(half + 1) * 512],
                        ALU.mult, ALU.add, [py, gatew, acc], [acc])
        for tl in range(nt_):
            ti = p * 8 + tl
            layer_norm(otile, otile[:], acc, acc[:, tl, :], ln2row[:, 0, :], ln2row[:, 1, :], ln2row)
            dma(out_d[ti * 128:(ti + 1) * 128, :], otile[:], [otile], [dout], eng="pool")

    return finish()


def _consts():
    k = np.arange(128)
    ident = np.eye(128, dtype=np.float32)
    same = (k[:, None] // 64) == (k[None, :] // 64)
    tri2 = (same & (k[:, None] <= k[None, :])).astype(np.float32)
    u2 = (same & (k[:, None] > k[None, :])).astype(np.float32)
    ebig = ((np.arange(S)[None, :] // 64) == k[:, None]).astype(np.float32)
    c = np.arange(512)
    j = np.arange(128)
    ovl = ((c[:, None] * 16 < j[None, :] * 64 + 64) & (j[None, :] * 64 < c[:, None] * 16 + 32)).astype(np.float32)
    ovl[511] = 0.0
    ovl = ovl.reshape(4, 128, 128).transpose(1, 0, 2)
    inv = (500000.0 ** (-np.arange(0, 16, 2, dtype=np.float32) / 16)).astype(np.float32)
    inv = np.broadcast_to(inv[None, :], (128, 8)).copy()
    return dict(c_ident=ident, c_tri2=tri2, c_u2=u2, c_ebig=ebig, c_ovl=np.ascontiguousarray(ovl), c_inv=inv)


def _core_consts(h):
    k = np.arange(128)
    tri = np.where(k[:, None] <= k[None, :], 0.0, NEG).astype(np.float32)
    strict = np.where(k[:, None] > k[None, :], 0.0, NEG).astype(np.float32)
    zeros = np.zeros((128, 128), np.float32)
    negs = np.full((128, 128), NEG, np.float32)
    diag = np.stack([tri, negs] if h == 0 else [zeros, tri], axis=1)
    if h == 0:
        win = [strict, zeros, zeros, zeros, tri, negs]
    else:
        win = [negs, strict, zeros, zeros, zeros, tri]
    win = np.stack(win, axis=1)
    cmpm = np.zeros((NI, 128, 4, 128), np.float32)
    impc = np.zeros((NI, 128, 2, 128), np.float32)
    cl = np.arange(128)
    jb = np.arange(128)
    for i in range(NI):
        qb = 2 * i + h
        tpos = qb * 128 + np.arange(128)
        for cc in range(4):
            cend = 16 * (cc * 128 + cl) + 31
            valid = (cend[:, None] <= tpos[None, :]) & ((cc * 128 + cl)[:, None] < 511)
            cmpm[i, :, cc, :] = valid
        cur = tpos // 64
        dist = cur[:, None] - jb[None, :]
        causal = dist >= 0
        forced = (jb[None, :] == 0) | (causal & (dist < 2))
        impc[i, :, 0, :] = causal
        impc[i, :, 1, :] = np.where(causal, 1000.0 * forced, -1.0)
    return dict(c_diag=diag, c_win=win, c_cmpm=cmpm, c_imp=impc, c_h=np.full((128, 1), float(h), np.float32))


_CACHE = {}


def kernel(x, positions, w_in, pe_cmp, w_ck1, w_ck2, w_cv1, w_cv2, hg_lb, hg_norm_g, w_o, ln1_g, ln1_b,
           w_router, b_router, w_gate_up, b_gate_up, w_down, b_down, ln2_g, ln2_b, _n_i=NI, _n_exp=32, _dbg=False):
    f = lambda a: np.ascontiguousarray(np.asarray(a))
    x = f(x); positions = f(positions); w_in = f(w_in)[0]
    sizes = (512, 128, 128, 128, 128, 128, 128, 24, 512, 512, 512, 512)
    offs = np.concatenate([[0], np.cumsum(sizes)])
    (q_a, k_c, v_c, k_s, v_s, k_w, v_w, g_a, q_h, f_h, i_h, g_h) = [w_in[:, offs[n]:offs[n + 1]] for n in range(12)]
    q_r = q_a.reshape(D, 2, 4, 64).transpose(0, 2, 1, 3).reshape(D, 512)
    wglob = np.concatenate([k_s, k_w, v_s, v_w, k_c, v_c, f_h, i_h], axis=1)
    wown = np.concatenate([q_r, g_a, q_h, f_h, i_h, g_h], axis=1)
    ck = lambda w: np.ascontiguousarray(w.reshape(8, 128, -1))
    cst = _consts()
    shared = dict(
        w_glob=ck(wglob), w_own=ck(wown),
        w_c1=np.stack([np.tile(f(w)[0].reshape(32, 64, 256).transpose(1, 0, 2).reshape(64, 32 * 256), (2, 1)) for w in (w_ck1, w_cv1)]),
        w_c2=np.stack([f(w)[0].reshape(2, 128, 64).transpose(1, 0, 2) for w in (w_ck2, w_cv2)]),
        peT=np.tile(f(pe_cmp)[0].T, (2, 1)),
        hg_lb=f(hg_lb), hg_ng=f(hg_norm_g),
        w_o=ck(f(w_o)[0]),
        lnp=np.stack([f(ln1_g)[0], f(ln1_b)[0], f(ln2_g)[0], f(ln2_b)[0]]),
        w_r=ck(f(w_router)[0]), b_r=f(b_router),
        w_gu=f(w_gate_up)[0].reshape(32, 8, 128, 2048)[:_n_exp],
        b_gu=np.ascontiguousarray(f(b_gate_up)[0].reshape(32, 16, 128).transpose(0, 2, 1))[:_n_exp],
        w_dn=f(w_down)[0].reshape(32, 8, 128, D)[:_n_exp],
        b_dn=f(b_down)[0].reshape(32, 1, D)[:_n_exp],
        **cst,
    )
    shared = {k: np.ascontiguousarray(v, dtype=np.float32) for k, v in shared.items()}
    in_maps = []
    for c in range(8):
        b, h = c // 2, c % 2
        xb = x[b]
        own_rows = (np.arange(NI)[:, None] * 256 + h * 128 + np.arange(128)[None, :]).reshape(-1)
        xo = xb[own_rows]
        pos = positions[b].astype(np.int32)
        cend = 16 * np.arange(512) + 31
        pc = np.where(cend < S, pos[np.minimum(cend, S - 1)], 0).astype(np.int32)
        m = dict(shared)
        m.update(
            xT_all=np.ascontiguousarray(xb.T.reshape(8, 128, S)),
            xT_own=np.ascontiguousarray(xo.T.reshape(8, 128, NI * 128)),
            x_own=np.ascontiguousarray(xo),
            pos_all=np.ascontiguousarray(pos.reshape(64, 128).T),
            pos_own=np.ascontiguousarray(pos[own_rows].reshape(32, 128).T),
            pos_cmp=np.ascontiguousarray(pc.reshape(4, 128).T),
        )
        cc = _core_consts(h)
        m.update({k: np.ascontiguousarray(v, dtype=np.float32) for k, v in cc.items()})
        in_maps.append(m)
    key = (_n_i, _n_exp, _dbg)
    nc = build_program(_n_i, _n_exp, _dbg)
    res = run_bass_kernel_spmd(nc, in_maps, core_ids=list(range(8)))
    out = np.zeros((4, S, D), np.float32)
    for c in range(8):
        b, h = c // 2, c % 2
        own_rows = (np.arange(NI)[:, None] * 256 + h * 128 + np.arange(128)[None, :]).reshape(-1)
        out[b, own_rows] = res.results[c]["out"]
    if _dbg:
        kernel.dbg = [res.results[c]["dbg"] for c in range(8)]
    return out
```
